# Optimizing a Trainium2 kernel written in Bass

```python
import jax
import jax.numpy as jnp
from jax import lax
import numpy as np

D_MODEL = 1024
BATCH = 16
SEQ = 2048
DEPTH = 2

N_GROUPS = 4
HEADS = 4
HEAD_DIM = 64
D_GROUP = HEADS * HEAD_DIM
D_MIX = N_GROUPS * D_GROUP
D_FF = 2816
Q_BLOCK = 128
ROPE_THETA = 10000.0
MLA_Q_RANK = 256
MLA_KV_RANK = 128
MLA_D_NOPE = 64
MLA_D_ROPE = 32
MLA_D_V = 64
NSA_CMP_LEN = 32
NSA_CMP_STRIDE = 16
NSA_SEL_LEN = 64
NSA_N_SEL = 8
NSA_N_INIT = 1
NSA_N_LOCAL = 2
NSA_WINDOW = 512
DSA_TOPK = 256
DSA_IDX_HEADS = 8
DSA_IDX_DIM = 32
DN_ALPHA = (2.0 * DEPTH) ** 0.25
DN_BETA = (8.0 * DEPTH) ** -0.25
LN_EPS = 1e-5
RMS_EPS = 1e-6
NEG_INF = -1e30
FORCE_SCORE = 1e4

IN_SPLITS = (
    ('mla_cq', MLA_Q_RANK), ('mla_ckv', MLA_KV_RANK), ('mla_krope', MLA_D_ROPE),
    ('nsa_q', D_GROUP), ('nsa_k_cmp', HEAD_DIM), ('nsa_v_cmp', HEAD_DIM),
    ('nsa_k_slc', HEAD_DIM), ('nsa_v_slc', HEAD_DIM), ('nsa_k_win', HEAD_DIM),
    ('nsa_v_win', HEAD_DIM), ('nsa_gate', 3 * HEADS),
    ('dsa_q', D_GROUP), ('dsa_k', HEAD_DIM), ('dsa_v', HEAD_DIM),
    ('idx_q', DSA_IDX_HEADS * DSA_IDX_DIM), ('idx_k', DSA_IDX_DIM), ('idx_w', DSA_IDX_HEADS),
    ('sb_q', D_GROUP), ('sb_k', D_GROUP), ('sb_v', D_GROUP),
)
D_IN = sum(w for _, w in IN_SPLITS)

kernel_name = 'hybrid_mla_nsa_dsa_stickbreak_block'


def layer_norm(x, g, b):
    xf = x.astype(jnp.float32)
    mu = jnp.mean(xf, -1, keepdims=True)
    var = jnp.mean(jnp.square(xf - mu), -1, keepdims=True)
    return ((xf - mu) * lax.rsqrt(var + LN_EPS) * g + b).astype(x.dtype)


def rms_norm(x, g):
    xf = x.astype(jnp.float32)
    return (xf * lax.rsqrt(jnp.mean(xf * xf, -1, keepdims=True) + RMS_EPS) * g).astype(x.dtype)


def swiglu(x, w1, w3, w2):
    return (jax.nn.silu(x @ w1) * (x @ w3)) @ w2


def rope_tables(seq, dim):
    inv = ROPE_THETA ** (-jnp.arange(0, dim, 2, dtype=jnp.float32) / dim)
    ang = jnp.arange(seq, dtype=jnp.float32)[:, None] * inv[None, :]
    return jnp.cos(ang), jnp.sin(ang)


def apply_rope(z, cos, sin):
    shape = (cos.shape[0],) + (1,) * (z.ndim - 3) + (cos.shape[1],)
    c = cos.reshape(shape).astype(z.dtype)
    s = sin.reshape(shape).astype(z.dtype)
    z1, z2 = jnp.split(z, 2, axis=-1)
    return jnp.concatenate([z1 * c - z2 * s, z1 * s + z2 * c], axis=-1)


def masked_softmax(s, mask):
    s = jnp.where(mask, s, NEG_INF)
    e = jnp.where(mask, jnp.exp(s - jnp.max(s, -1, keepdims=True)), 0.0)
    return e / jnp.maximum(jnp.sum(e, -1, keepdims=True), 1e-30)


def unblock(y):
    nb, b, qb = y.shape[:3]
    return jnp.moveaxis(y, 0, 1).reshape((b, nb * qb) + y.shape[3:])


def split_columns(h):
    parts = {}
    off = 0
    for name, width in IN_SPLITS:
        parts[name] = h[..., off:off + width]
        off += width
    return parts


def mla_mixer(c_q, c_kv, k_rope, q_norm_g, w_uq, kv_norm_g, w_ukv, cos_r, sin_r):
    B, S, _ = c_q.shape
    q = (rms_norm(c_q, q_norm_g) @ w_uq).reshape(B, S, HEADS, MLA_D_NOPE + MLA_D_ROPE)
    q_nope = q[..., :MLA_D_NOPE]
    q_pe = apply_rope(q[..., MLA_D_NOPE:], cos_r, sin_r)
    kv = (rms_norm(c_kv, kv_norm_g) @ w_ukv).reshape(B, S, HEADS, MLA_D_NOPE + MLA_D_V)
    k_nope = kv[..., :MLA_D_NOPE]
    v = kv[..., MLA_D_NOPE:]
    k_pe = apply_rope(k_rope, cos_r, sin_r)
    scale = (MLA_D_NOPE + MLA_D_ROPE) ** -0.5
    k_pos = jnp.arange(S)

    def block(i):
        q0 = i * Q_BLOCK
        qn = lax.dynamic_slice_in_dim(q_nope, q0, Q_BLOCK, 1)
        qp = lax.dynamic_slice_in_dim(q_pe, q0, Q_BLOCK, 1)
        s = jnp.einsum('bqhd,bkhd->bhqk', qn, k_nope) + jnp.einsum('bqhd,bkd->bhqk', qp, k_pe)
        q_pos = q0 + jnp.arange(Q_BLOCK)
        p = masked_softmax(s.astype(jnp.float32) * scale, k_pos[None, :] <= q_pos[:, None])
        return jnp.einsum('bhqk,bkhd->bqhd', p.astype(v.dtype), v)

    return unblock(lax.map(block, jnp.arange(S // Q_BLOCK))).reshape(B, S, D_GROUP)


def nsa_mixer(q, k_cmp, v_cmp, k_slc, v_slc, k_win, v_win, gate_logits,
              cmp_pos, wk1, wk2, wv1, wv2, cos_h, sin_h):
    B, S, _ = q.shape
    q = apply_rope(q.reshape(B, S, HEADS, HEAD_DIM), cos_h, sin_h)
    k_cmp = apply_rope(k_cmp, cos_h, sin_h)
    k_slc = apply_rope(k_slc, cos_h, sin_h)
    k_win = apply_rope(k_win, cos_h, sin_h)
    scale = HEAD_DIM ** -0.5
    t_pos = jnp.arange(S)

    n_cmp = (S - NSA_CMP_LEN) // NSA_CMP_STRIDE + 1
    tok = np.arange(n_cmp)[:, None] * NSA_CMP_STRIDE + np.arange(NSA_CMP_LEN)[None, :]

    def compress(z, w1, w2):
        blk = (z[:, tok] + cmp_pos).reshape(B, n_cmp, NSA_CMP_LEN * HEAD_DIM)
        return jax.nn.gelu(blk @ w1) @ w2

    kc = compress(k_cmp, wk1, wk2)
    vc = compress(v_cmp, wv1, wv2)
    s_c = jnp.einsum('bshd,bnd->bhsn', q, kc).astype(jnp.float32) * scale
    p_c = masked_softmax(s_c, tok[:, -1][None, :] <= t_pos[:, None])
    o_cmp = jnp.einsum('bhsn,bnd->bshd', p_c.astype(vc.dtype), vc)

    n_slc = S // NSA_SEL_LEN
    slc_start = np.arange(n_slc) * NSA_SEL_LEN
    overlap = ((tok[:, :1] <= slc_start[None, :] + NSA_SEL_LEN - 1)
               & (tok[:, -1:] >= slc_start[None, :])).astype(np.float32)
    imp = jnp.einsum('bhsn,nj->bsj', p_c, jnp.asarray(overlap))
    cur = (t_pos // NSA_SEL_LEN)[:, None]
    jb = jnp.arange(n_slc)[None, :]
    forced = (jb < NSA_N_INIT) | ((jb <= cur) & (jb > cur - NSA_N_LOCAL))
    imp = jnp.where(forced, FORCE_SCORE, jnp.where(jb <= cur, imp, NEG_INF))
    n_top = min(NSA_N_SEL, n_slc)
    _, sel = lax.top_k(imp, n_top)
    k_blk = k_slc.reshape(B, n_slc, NSA_SEL_LEN, HEAD_DIM)
    v_blk = v_slc.reshape(B, n_slc, NSA_SEL_LEN, HEAD_DIM)
    kw = jnp.pad(k_win, ((0, 0), (NSA_WINDOW, 0), (0, 0)))
    vw = jnp.pad(v_win, ((0, 0), (NSA_WINDOW, 0), (0, 0)))
    b_idx = jnp.arange(B)[:, None, None]
    n_sel_tok = n_top * NSA_SEL_LEN

    def block(i):
        q0 = i * Q_BLOCK
        qb = lax.dynamic_slice_in_dim(q, q0, Q_BLOCK, 1)
        q_pos = q0 + jnp.arange(Q_BLOCK)
        idx = lax.dynamic_slice_in_dim(sel, q0, Q_BLOCK, 1)
        kg = k_blk[b_idx, idx].reshape(B, Q_BLOCK, n_sel_tok, HEAD_DIM)
        vg = v_blk[b_idx, idx].reshape(B, Q_BLOCK, n_sel_tok, HEAD_DIM)
        g_pos = (idx[..., None] * NSA_SEL_LEN + jnp.arange(NSA_SEL_LEN)).reshape(B, Q_BLOCK, n_sel_tok)
        s = jnp.einsum('bqhd,bqkd->bhqk', qb, kg).astype(jnp.float32) * scale
        p = masked_softmax(s, (g_pos <= q_pos[None, :, None])[:, None])
        o_s = jnp.einsum('bhqk,bqkd->bqhd', p.astype(vg.dtype), vg)
        kb = lax.dynamic_slice_in_dim(kw, q0, NSA_WINDOW + Q_BLOCK, 1)
        vb = lax.dynamic_slice_in_dim(vw, q0, NSA_WINDOW + Q_BLOCK, 1)
        w_pos = q0 - NSA_WINDOW + jnp.arange(NSA_WINDOW + Q_BLOCK)
        dist = q_pos[:, None] - w_pos[None, :]
        s = jnp.einsum('bqhd,bkd->bhqk', qb, kb).astype(jnp.float32) * scale
        p = masked_softmax(s, (dist >= 0) & (dist < NSA_WINDOW) & (w_pos[None, :] >= 0))
        o_w = jnp.einsum('bhqk,bkd->bqhd', p.astype(vb.dtype), vb)
        return o_s, o_w

    o_slc, o_win = lax.map(block, jnp.arange(S // Q_BLOCK))
    o_slc = unblock(o_slc)
    o_win = unblock(o_win)
    g = jax.nn.sigmoid(gate_logits).reshape(B, S, 3, HEADS)[..., None]
    o = g[:, :, 0] * o_cmp + g[:, :, 1] * o_slc + g[:, :, 2] * o_win
    return o.reshape(B, S, D_GROUP)


def dsa_mixer(q, k, v, q_idx, k_idx, w_idx, cos_h, sin_h, cos_i, sin_i):
    B, S, _ = q.shape
    q = apply_rope(q.reshape(B, S, HEADS, HEAD_DIM), cos_h, sin_h)
    k = apply_rope(k, cos_h, sin_h)
    q_idx = apply_rope(q_idx.reshape(B, S, DSA_IDX_HEADS, DSA_IDX_DIM), cos_i, sin_i)
    k_idx = apply_rope(k_idx, cos_i, sin_i)
    w_idx = w_idx.astype(jnp.float32) * (DSA_IDX_HEADS * DSA_IDX_DIM) ** -0.5
    scale = HEAD_DIM ** -0.5
    n_top = min(DSA_TOPK, S // 4)
    k_pos = jnp.arange(S)
    b_idx = jnp.arange(B)[:, None, None]

    def block(i):
        q0 = i * Q_BLOCK
        q_pos = q0 + jnp.arange(Q_BLOCK)
        qi = lax.dynamic_slice_in_dim(q_idx, q0, Q_BLOCK, 1)
        wi = lax.dynamic_slice_in_dim(w_idx, q0, Q_BLOCK, 1)
        rel = jax.nn.relu(jnp.einsum('bqhd,bsd->bqhs', qi, k_idx).astype(jnp.float32))
        score = jnp.einsum('bqh,bqhs->bqs', wi, rel)
        score = jnp.where(k_pos[None, None, :] <= q_pos[None, :, None], score, NEG_INF)
        _, idx = lax.top_k(score, n_top)
        kg = k[b_idx, idx]
        vg = v[b_idx, idx]
        qb = lax.dynamic_slice_in_dim(q, q0, Q_BLOCK, 1)
        s = jnp.einsum('bqhd,bqkd->bhqk', qb, kg).astype(jnp.float32) * scale
        p = masked_softmax(s, (idx <= q_pos[None, :, None])[:, None])
        return jnp.einsum('bhqk,bqkd->bqhd', p.astype(vg.dtype), vg)

    return unblock(lax.map(block, jnp.arange(S // Q_BLOCK))).reshape(B, S, D_GROUP)


def stick_breaking_mixer(q, k, v):
    B, S, _ = q.shape
    q = q.reshape(B, S, HEADS, HEAD_DIM)
    k = k.reshape(B, S, HEADS, HEAD_DIM)
    v = v.reshape(B, S, HEADS, HEAD_DIM)
    scale = HEAD_DIM ** -0.5
    k_pos = jnp.arange(S)

    def block(i):
        q0 = i * Q_BLOCK
        qb = lax.dynamic_slice_in_dim(q, q0, Q_BLOCK, 1)
        q_pos = q0 + jnp.arange(Q_BLOCK)
        z = jnp.einsum('bqhd,bkhd->bhqk', qb, k).astype(jnp.float32) * scale
        mask = k_pos[None, :] < q_pos[:, None]
        log_1m = jnp.where(mask, jax.nn.log_sigmoid(-z), 0.0)
        after = lax.cumsum(log_1m, axis=3, reverse=True) - log_1m
        a = jnp.where(mask, jnp.exp(jax.nn.log_sigmoid(z) + after), 0.0)
        return jnp.einsum('bhqk,bkhd->bqhd', a.astype(v.dtype), v)

    return unblock(lax.map(block, jnp.arange(S // Q_BLOCK))).reshape(B, S, D_GROUP)


def hybrid_mixer(x, w_in, mla_q_norm, mla_w_uq, mla_kv_norm, mla_w_ukv, nsa_cmp_pos,
                 nsa_cmp_wk1, nsa_cmp_wk2, nsa_cmp_wv1, nsa_cmp_wv2, group_norm_g, w_out,
                 cos_h, sin_h, cos_r, sin_r, cos_i, sin_i):
    B, S, _ = x.shape
    h = split_columns(x @ w_in)
    y_a = mla_mixer(h['mla_cq'], h['mla_ckv'], h['mla_krope'], mla_q_norm, mla_w_uq,
                    mla_kv_norm, mla_w_ukv, cos_r, sin_r)
    y_b = nsa_mixer(h['nsa_q'], h['nsa_k_cmp'], h['nsa_v_cmp'], h['nsa_k_slc'], h['nsa_v_slc'],
                    h['nsa_k_win'], h['nsa_v_win'], h['nsa_gate'], nsa_cmp_pos,
                    nsa_cmp_wk1, nsa_cmp_wk2, nsa_cmp_wv1, nsa_cmp_wv2, cos_h, sin_h)
    y_c = dsa_mixer(h['dsa_q'], h['dsa_k'], h['dsa_v'], h['idx_q'], h['idx_k'], h['idx_w'],
                    cos_h, sin_h, cos_i, sin_i)
    y_d = stick_breaking_mixer(h['sb_q'], h['sb_k'], h['sb_v'])
    y = jnp.concatenate([y_a, y_b, y_c, y_d], axis=-1).reshape(B, S, N_GROUPS, D_GROUP)
    y = rms_norm(y, group_norm_g.reshape(N_GROUPS, D_GROUP)).reshape(B, S, D_MIX)
    return y @ w_out


def setup_inputs(seed: int = 0) -> dict:
    key = jax.random.key(seed)
    keys = jax.random.split(key, 32)
    counter = [0]

    def nrm(shape, scale):
        k = keys[counter[0]]
        counter[0] += 1
        return jax.random.normal(k, shape, jnp.float32) * scale

    def gain(shape):
        return 1.0 + nrm(shape, 0.02)

    L, D = DEPTH, D_MODEL
    return {
        'x': nrm((BATCH, SEQ, D), 1.0),
        'ln1_g': gain((L, D)),
        'ln1_b': nrm((L, D), 0.02),
        'ffn1_w1': nrm((L, D, D_FF), D ** -0.5),
        'ffn1_w3': nrm((L, D, D_FF), D ** -0.5),
        'ffn1_w2': nrm((L, D_FF, D), DN_BETA * D_FF ** -0.5),
        'w_in': nrm((L, D, D_IN), D ** -0.5),
        'mla_q_norm': gain((L, MLA_Q_RANK)),
        'mla_w_uq': nrm((L, MLA_Q_RANK, HEADS * (MLA_D_NOPE + MLA_D_ROPE)), MLA_Q_RANK ** -0.5),
        'mla_kv_norm': gain((L, MLA_KV_RANK)),
        'mla_w_ukv': nrm((L, MLA_KV_RANK, HEADS * (MLA_D_NOPE + MLA_D_V)), MLA_KV_RANK ** -0.5),
        'nsa_cmp_pos': nrm((L, NSA_CMP_LEN, HEAD_DIM), 0.1),
        'nsa_cmp_wk1': nrm((L, NSA_CMP_LEN * HEAD_DIM, HEAD_DIM), (NSA_CMP_LEN * HEAD_DIM) ** -0.5),
        'nsa_cmp_wk2': nrm((L, HEAD_DIM, HEAD_DIM), HEAD_DIM ** -0.5),
        'nsa_cmp_wv1': nrm((L, NSA_CMP_LEN * HEAD_DIM, HEAD_DIM), (NSA_CMP_LEN * HEAD_DIM) ** -0.5),
        'nsa_cmp_wv2': nrm((L, HEAD_DIM, HEAD_DIM), HEAD_DIM ** -0.5),
        'group_norm_g': gain((L, D_MIX)),
        'w_out': nrm((L, D_MIX, D), DN_BETA * D_MIX ** -0.5),
        'ln2_g': gain((L, D)),
        'ln2_b': nrm((L, D), 0.02),
        'ffn2_w1': nrm((L, D, D_FF), D ** -0.5),
        'ffn2_w3': nrm((L, D, D_FF), D ** -0.5),
        'ffn2_w2': nrm((L, D_FF, D), DN_BETA * D_FF ** -0.5),
        'ln3_g': gain((L, D)),
        'ln3_b': nrm((L, D), 0.02),
    }


def reference(x, ln1_g, ln1_b, ffn1_w1, ffn1_w3, ffn1_w2, w_in, mla_q_norm, mla_w_uq,
              mla_kv_norm, mla_w_ukv, nsa_cmp_pos, nsa_cmp_wk1, nsa_cmp_wk2, nsa_cmp_wv1,
              nsa_cmp_wv2, group_norm_g, w_out, ln2_g, ln2_b, ffn2_w1, ffn2_w3, ffn2_w2,
              ln3_g, ln3_b):
    S = x.shape[1]
    cos_h, sin_h = rope_tables(S, HEAD_DIM)
    cos_r, sin_r = rope_tables(S, MLA_D_ROPE)
    cos_i, sin_i = rope_tables(S, DSA_IDX_DIM)
    for l in range(DEPTH):
        x = layer_norm(DN_ALPHA * x + 0.5 * swiglu(x, ffn1_w1[l], ffn1_w3[l], ffn1_w2[l]),
                       ln1_g[l], ln1_b[l])
        y = hybrid_mixer(x, w_in[l], mla_q_norm[l], mla_w_uq[l], mla_kv_norm[l], mla_w_ukv[l],
                         nsa_cmp_pos[l], nsa_cmp_wk1[l], nsa_cmp_wk2[l], nsa_cmp_wv1[l],
                         nsa_cmp_wv2[l], group_norm_g[l], w_out[l],
                         cos_h, sin_h, cos_r, sin_r, cos_i, sin_i)
        x = layer_norm(DN_ALPHA * x + y, ln2_g[l], ln2_b[l])
        x = layer_norm(DN_ALPHA * x + 0.5 * swiglu(x, ffn2_w1[l], ffn2_w3[l], ffn2_w2[l]),
                       ln3_g[l], ln3_b[l])
    return x
```

```python
import contextlib
import numpy as np
import concourse.bass as bass
import concourse.mybir as mybir
from concourse.bass_utils import run_bass_kernel_spmd

F32 = mybir.dt.float32
BF16 = mybir.dt.bfloat16
AF = mybir.ActivationFunctionType
ALU = mybir.AluOpType

SEQ = 2048
D = 1024
DFF = 2816
NT = 16
DEPTH = 2
NCORES = 8
CORES_PER_LAUNCH = 2
D_IN = 2516
ALPHA = float((2.0 * DEPTH) ** 0.25)
LN_EPS = 1e-5
RMS_EPS = 1e-6
BIG = 30000.0
NIT = 16
SCR_BYTES = 96 * 1024

OFF = {}
_o = 0
for _n, _w in (('mla_cq', 256), ('mla_ckv', 128), ('mla_krope', 32), ('nsa_q', 256), ('nsa_k_cmp', 64),
               ('nsa_v_cmp', 64), ('nsa_k_slc', 64), ('nsa_v_slc', 64), ('nsa_k_win', 64), ('nsa_v_win', 64),
               ('nsa_gate', 12), ('dsa_q', 256), ('dsa_k', 64), ('dsa_v', 64), ('idx_q', 256), ('idx_k', 32),
               ('idx_w', 8), ('sb_q', 256), ('sb_k', 256), ('sb_v', 256)):
    OFF[_n] = _o
    _o += _w

ENGS = ("pe", "act", "dve", "pool", "sp")
EPOCH = 4000
NRING = 24
RINGSZ = {"sp": 16, "pool": 3}


class Sched:
    def __init__(self, nc):
        self.nc = nc
        self.ops = []
        self.state = {}
        self.ndma = {e: 0 for e in ENGS}
        self.last = {e: None for e in ENGS}
        self.dmas_since_fence = []

    def _add(self, eng, fn, reads, writes, dma, extra=()):
        i = len(self.ops)
        deps = set()
        for r in reads:
            st = self.state.get(r)
            if st is not None and st[0] is not None:
                deps.add((st[0], "raw"))
        for w in writes:
            st = self.state.get(w)
            if st is not None:
                if st[0] is not None:
                    deps.add((st[0], "waw"))
                for rd in st[1]:
                    deps.add((rd, "war"))
        real = set(extra)
        for (a, kind) in deps:
            A = self.ops[a]
            if A["eng"] == eng and not A["dma"] and not dma:
                if eng == "pe":
                    continue
            real.add(a)
        best = {}
        pruned = set()
        for a in real:
            A = self.ops[a]
            if A["dma"]:
                pruned.add(a)
            else:
                if best.get(A["eng"], -1) < a:
                    best[A["eng"]] = a
        pruned.update(best.values())
        real = pruned
        op = dict(eng=eng, fn=fn, dma=dma, deps=real, flag=False, slot=None)
        if dma:
            op["slot"] = self.ndma[eng]
            self.ndma[eng] += 1
            self.dmas_since_fence.append(i)
        for a in real:
            self.ops[a]["flag"] = True
        self.ops.append(op)
        self.last[eng] = i
        for r in reads:
            st = self.state.setdefault(r, [None, []])
            st[1].append(i)
        for w in writes:
            self.state[w] = [i, []]
        return i

    def op(self, eng, fn, reads=(), writes=()):
        return self._add(eng, fn, tuple(reads), tuple(writes), False)

    def dma(self, eng, out, in_, reads=(), writes=()):
        return self._add(eng, lambda e: e.dma_start(out=out, in_=in_), tuple(reads), tuple(writes), True)

    def mm(self, out, lhsT, rhs, start, stop, reads, writes, skip=False):
        if skip:
            return self.op("pe", lambda e: e.matmul(out, lhsT=lhsT, rhs=rhs, start=start, stop=stop, skip_group_check=True), reads, writes)
        return self.op("pe", lambda e: e.matmul(out, lhsT=lhsT, rhs=rhs, start=start, stop=stop), reads, writes)

    def tr(self, out, in_, ident, reads, writes):
        return self.op("pe", lambda e: e.transpose(out=out, in_=in_, identity=ident), reads, writes)

    def act(self, out, in_, func, reads, writes, scale=1.0, bias=None, accum=None):
        def f(e):
            kw = {}
            if bias is not None:
                kw["bias"] = bias
            if accum is not None:
                kw["accum_out"] = accum
            return e.activation(out=out, in_=in_, func=func, scale=scale, **kw)
        return self.op("act", f, reads, writes)

    def tt(self, eng, out, in0, in1, op, reads, writes):
        return self.op(eng, lambda e: e.tensor_tensor(out=out, in0=in0, in1=in1, op=op), reads, writes)

    def ts(self, eng, out, in0, s1, s2, op0, op1, reads, writes, accum=None):
        def f(e):
            kw = {}
            if accum is not None:
                kw["accum_out"] = accum
            if op1 is None:
                return e.tensor_scalar(out=out, in0=in0, scalar1=s1, scalar2=None, op0=op0, **kw)
            return e.tensor_scalar(out=out, in0=in0, scalar1=s1, scalar2=s2, op0=op0, op1=op1, **kw)
        return self.op(eng, f, reads, writes)

    def stt(self, out, in0, scalar, in1, op0, op1, reads, writes, accum=None):
        def f(e):
            kw = {}
            if accum is not None:
                kw["accum_out"] = accum
            return e.scalar_tensor_tensor(out=out, in0=in0, scalar=scalar, in1=in1, op0=op0, op1=op1, **kw)
        return self.op("dve", f, reads, writes)

    def copy(self, eng, out, in_, reads, writes):
        if eng == "act":
            return self.op("act", lambda e: e.activation(out=out, in_=in_, func=AF.Copy), reads, writes)
        return self.op(eng, lambda e: e.tensor_copy(out=out, in_=in_), reads, writes)

    def memset(self, eng, out, val, writes):
        return self.op(eng, lambda e: e.memset(out, val), (), writes)

    def recip(self, out, in_, reads, writes):
        return self.op("dve", lambda e: e.reciprocal(out=out, in_=in_), reads, writes)

    def fence(self):
        lasts = [v for v in self.last.values() if v is not None]
        lasts = [v for v in lasts if not self.ops[v]["dma"]]
        dm = list(self.dmas_since_fence)
        lastc = {}
        for i in range(len(self.ops) - 1, -1, -1):
            o = self.ops[i]
            if not o["dma"] and o["eng"] not in lastc:
                lastc[o["eng"]] = i
            if len(lastc) == len(ENGS):
                break
        for e in ENGS:
            extra = set(dm)
            for e2, i2 in lastc.items():
                extra.add(i2)
            self._add(e, lambda eng: eng.nop(), (), (), False, extra=extra)
        self.state = {}
        self.dmas_since_fence = []

    def emit(self):
        nc = self.nc
        cnt = {e: 0 for e in ENGS}
        for op in self.ops:
            if not op["dma"] and op["flag"]:
                cnt[op["eng"]] += 1
                op["cnt"] = cnt[op["eng"]]
        nsem = {e: max(1, (cnt[e] + EPOCH - 1) // EPOCH) for e in ENGS}
        with contextlib.ExitStack() as es:
            esem = {e: [es.enter_context(nc.semaphore(f"s_{e}{k}")) for k in range(nsem[e])] for e in ENGS}
            ring = {e: [es.enter_context(nc.semaphore(f"ring_{e}{k}")) for k in range(RINGSZ[e])] for e in ("sp", "pool")}
            block = es.enter_context(nc.Block())
            per_eng = {e: [] for e in ENGS}
            for i, op in enumerate(self.ops):
                per_eng[op["eng"]].append(i)
            ops = self.ops

            def gen(ename):
                def body(eng):
                    waited = {}
                    for i in per_eng[ename]:
                        op = ops[i]
                        need = {}
                        for a in op["deps"]:
                            A = ops[a]
                            if A["dma"]:
                                nr = RINGSZ[A["eng"]]
                                key = ("r" + A["eng"], A["slot"] % nr)
                                val = 16 * (A["slot"] // nr + 1)
                            else:
                                c = A["cnt"] - 1
                                key = (A["eng"], c // EPOCH)
                                val = c % EPOCH + 1
                            if need.get(key, 0) < val:
                                need[key] = val
                        if op["dma"]:
                            s = op["slot"]
                            nr = RINGSZ[ename]
                            if s >= nr:
                                key = ("r" + ename, s % nr)
                                val = 16 * (s // nr)
                                if need.get(key, 0) < val:
                                    need[key] = val
                        for key, val in need.items():
                            if waited.get(key, 0) >= val:
                                continue
                            waited[key] = val
                            sem = ring[key[0][1:]][key[1]] if key[0][0] == "r" else esem[key[0]][key[1]]
                            eng.wait_ge(sem, val)
                        ins = op["fn"](eng)
                        if op["dma"]:
                            ins.then_inc(ring[ename][op["slot"] % RINGSZ[ename]], 16)
                        elif op["flag"]:
                            c = op["cnt"] - 1
                            ins.then_inc(esem[ename][c // EPOCH], 1)
                    last = {}
                    for i in per_eng[ename]:
                        op = ops[i]
                        if op["dma"]:
                            last[op["slot"] % RINGSZ[ename]] = 16 * (op["slot"] // RINGSZ[ename] + 1)
                    for r, val in last.items():
                        if waited.get(("r" + ename, r), 0) < val:
                            eng.wait_ge(ring[ename][r], val)
                return body

            block.tensor(gen("pe"))
            block.scalar(gen("act"))
            block.vector(gen("dve"))
            block.gpsimd(gen("pool"))
            block.sync(gen("sp"))


class Carver:
    def __init__(self, scr, nbytes):
        self.scr = scr
        self.nbytes = nbytes
        self.off = 0
        self.marks = []
        self.peak = 0

    def mark(self):
        self.marks.append(self.off)

    def release(self):
        self.off = self.marks.pop()

    def get(self, shape, dt):
        n = 1
        for s in shape[1:]:
            n *= s
        esz = 4 if dt == F32 else 2
        nb = (n * esz + 31) // 32 * 32
        assert self.off + nb <= self.nbytes, f"scratch overflow {self.off}+{nb}>{self.nbytes}"
        ap = self.scr[:, self.off // 2:(self.off + n * esz) // 2]
        self.off += nb
        self.peak = max(self.peak, self.off)
        if dt == F32:
            ap = ap.bitcast(F32)
        if len(shape) == 3:
            ap = ap.rearrange("p (a b) -> p a b", b=shape[2])
        elif len(shape) == 4:
            ap = ap.rearrange("p (a b c) -> p a b c", b=shape[2], c=shape[3])
        if shape[0] < 128:
            ap = ap[0:shape[0]]
        return ap


def make_consts():
    c = {}
    k = np.arange(128)[:, None]
    q = np.arange(128)[None, :]
    masks = np.zeros((128, 3, 128), np.float32)
    masks[:, 0, :] = np.where(k > q, -BIG, 0.0)
    masks[:, 1, :] = np.where(k >= q, -BIG, 0.0)
    masks[:, 2, :] = np.where(k <= q, -BIG, 0.0)
    c["c_masks"] = masks
    c["c_ident"] = np.eye(128, dtype=np.float32)
    cums = np.zeros((128, 2, 128), np.float32)
    cums[:, 0, :] = np.where(k >= q, -1.0, 0.0)
    cums[:, 1, :] = -1.0
    c["c_cums"] = cums
    n = np.arange(128)[:, None]
    t = np.arange(SEQ)[None, :]
    cm = np.where(16 * n + 31 <= t, 0.0, -BIG).astype(np.float32)
    cm[127, :] = -BIG
    c["c_cmpmask"] = cm
    ex = np.zeros((32, 16, 128), np.float32)
    for kt in range(16):
        for s in range(128):
            ex[2 * kt + s // 64, kt, s] = 1.0
    c["c_expand"] = ex
    tt = np.arange(SEQ)
    cur = tt // 64
    jb = np.arange(32)[None, :]
    forced = (jb < 1) | ((jb <= cur[:, None]) & (jb > cur[:, None] - 2))
    causal = jb <= cur[:, None]
    keep = (causal & ~forced).astype(np.float32)
    add = np.where(forced, 1e4, np.where(causal, 0.0, -1e30)).astype(np.float32)
    c["c_selkeep"] = keep.reshape(16, 128, 32).transpose(1, 0, 2).copy()
    c["c_seladd"] = add.reshape(16, 128, 32).transpose(1, 0, 2).copy()
    nn = np.arange(127)
    tok0 = nn * 16
    tok1 = nn * 16 + 31
    ss = np.arange(32) * 64
    ov = ((tok0[:, None] <= ss[None, :] + 63) & (tok1[:, None] >= ss[None, :])).astype(np.float32)
    ovp = np.zeros((128, 32), np.float32)
    ovp[:127] = ov
    c["c_overlap"] = ovp

    def rope(dim):
        inv = (10000.0 ** (-np.arange(0, dim, 2, dtype=np.float32) / np.float32(dim))).astype(np.float32)
        ang = np.arange(SEQ, dtype=np.float32)[:, None] * inv[None, :]
        cs, sn = np.cos(ang).astype(np.float32), np.sin(ang).astype(np.float32)
        r = np.arange(128) % (dim // 2)
        return np.stack([cs[:, r].T, sn[:, r].T]).astype(np.float32)
    c["c_rope64"] = rope(64)
    c["c_rope32"] = rope(32)
    c["c_pow2"] = np.tile((0.5 ** np.arange(1, NIT + 1, dtype=np.float32))[None, :], (128, 1)).astype(np.float32)
    c["c_negtrif"] = np.where(q > k, -1e30, 0.0).astype(np.float32)
    return c


CONST_SHAPES = {
    "c_masks": [128, 3, 128], "c_ident": [128, 128], "c_cums": [128, 2, 128], "c_cmpmask": [128, SEQ],
    "c_expand": [32, 16, 128], "c_selkeep": [128, 16, 32], "c_seladd": [128, 16, 32], "c_overlap": [128, 32],
    "c_rope64": [2, 128, SEQ], "c_rope32": [2, 128, SEQ], "c_pow2": [128, NIT], "c_negtrif": [128, 128],
}

WEIGHT_SHAPES = {
    'ln1_g': [2, D], 'ln1_b': [2, D], 'ffn1_w1': [2, D, DFF], 'ffn1_w3': [2, D, DFF], 'ffn1_w2': [2, DFF, D],
    'w_in': [2, D, D_IN], 'mla_q_norm': [2, 256], 'mla_w_uq': [2, 256, 384], 'mla_kv_norm': [2, 128],
    'mla_w_ukv': [2, 128, 512], 'nsa_cmp_pos': [2, 32, 64], 'nsa_cmp_wk1': [2, 2048, 64],
    'nsa_cmp_wk2': [2, 64, 64], 'nsa_cmp_wv1': [2, 2048, 64], 'nsa_cmp_wv2': [2, 64, 64],
    'group_norm_g': [2, D], 'w_out': [2, D, D], 'ln2_g': [2, D], 'ln2_b': [2, D],
    'ffn2_w1': [2, D, DFF], 'ffn2_w3': [2, D, DFF], 'ffn2_w2': [2, DFF, D], 'ln3_g': [2, D], 'ln3_b': [2, D],
}


class Ctx:
    pass


def build(nseq=2, depth=DEPTH, phases=("ffn1", "mla", "nsa", "dsa", "sb", "ffn2"), taps=()):
    nc = bass.Bass("TRN2", target_bir_lowering=False)
    c = Ctx()
    c.nc = nc
    c.taps = {}
    dr = {}
    dr["x"] = nc.dram_tensor("x", [nseq * SEQ, D], F32, kind="ExternalInput").ap()
    for k, shp in WEIGHT_SHAPES.items():
        dr[k] = nc.dram_tensor(k, shp, F32, kind="ExternalInput").ap()
    for k, shp in CONST_SHAPES.items():
        dr[k] = nc.dram_tensor(k, shp, F32, kind="ExternalInput").ap()
    dr["out"] = nc.dram_tensor("out", [nseq * SEQ, D], F32, kind="ExternalOutput").ap()
    for (name, shp) in taps:
        c.taps[name] = nc.dram_tensor("tap_" + name, shp, F32, kind="ExternalOutput").ap()
    c.dr = dr

    with contextlib.ExitStack() as es:
        def sb(name, shape, dt):
            return es.enter_context(nc.sbuf_tensor("sb_" + name, shape, dt))

        def ps(name, shape, dt):
            return es.enter_context(nc.psum_tensor(name, shape, dt))

        c.x = sb("x", [128, NT, D], F32)
        c.xT = sb("xT", [128, 8, SEQ], BF16)
        c.ident = sb("ident", [128, 128], BF16)
        c.masks = sb("masks", [128, 3, 128], BF16)
        c.cums = sb("cums", [128, 2, 128], BF16)
        c.cmpmask = sb("cmpmask", [128, SEQ], BF16)
        c.expand = sb("expand", [32, 16, 128], BF16)
        c.selkeep = sb("selkeep", [128, 16, 32], F32)
        c.seladd = sb("seladd", [128, 16, 32], F32)
        c.pow2 = sb("pow2", [128, NIT], F32)
        c.negtrif = sb("negtrif", [128, 128], F32)
        c.mhalf = sb("mhalf", [128, 16], F32)
        c.one = sb("one", [128, 1], F32)
        scr = sb("scr", [128, SCR_BYTES // 2], BF16)
        c.A = Carver(scr, SCR_BYTES)
        c.P = [ps(f"ps{i}", [128, 512], F32) for i in range(7)]
        c.PT = ps("psT", [128, 1024], BF16)

        S = Sched(nc)
        c.S = S
        S.dma("pool", c.ident[:], dr["c_ident"], writes=["ident"])
        S.dma("pool", c.masks[:], dr["c_masks"], writes=["masks"])
        S.dma("pool", c.cums[:], dr["c_cums"], writes=["cums"])
        S.dma("pool", c.cmpmask[:], dr["c_cmpmask"], writes=["cmpmask"])
        S.dma("pool", c.expand[:], dr["c_expand"], writes=["expand"])
        S.dma("sp", c.selkeep[:], dr["c_selkeep"], writes=["selkeep"])
        S.dma("sp", c.seladd[:], dr["c_seladd"], writes=["seladd"])
        S.dma("sp", c.pow2[:], dr["c_pow2"], writes=["pow2"])
        S.dma("sp", c.negtrif[:], dr["c_negtrif"], writes=["negtrif"])
        S.op("pool", lambda e: e.memset(c.mhalf[:], -0.5), writes=["mhalf"])
        S.op("pool", lambda e: e.memset(c.one[:], 1.0), writes=["one"])
        S.fence()
        c.consts_res = ["ident", "masks", "cums", "cmpmask", "expand", "selkeep", "seladd", "pow2", "negtrif", "mhalf"]

        for s in range(nseq):
            load_x(c, s)
            for l in range(depth):
                last = (l == depth - 1)
                if "ffn1" in phases:
                    ffn(c, l, "ffn1", "ln1", out_rows=None)
                    tap(c, "x1", l)
                mixer(c, l, phases)
                tap(c, "x2", l)
                if "ffn2" in phases:
                    ffn(c, l, "ffn2", "ln3", out_rows=(s * SEQ if last else None))
                    tap(c, "x3", l)
            if "ffn2" not in phases:
                store_x(c, s)
        S.emit()
    return nc


def tap(c, name, l):
    key = f"{name}_{l}"
    if key in c.taps:
        S = c.S
        S.fence()
        for t in range(NT):
            S.dma("sp", c.taps[key][t * 128:(t + 1) * 128, :], c.x[:, t, :], reads=[("x", t)])
        S.fence()


def load_x(c, s):
    S = c.S
    S.fence()
    for t in range(NT):
        S.dma("sp", c.x[:, t, :], c.dr["x"][s * SEQ + t * 128: s * SEQ + (t + 1) * 128, :], writes=[("x", t)])
    A = c.A
    A.mark()
    xb = [A.get([128, D], BF16) for _ in range(2)]
    for t in range(NT):
        make_xT_tile(c, t, xb[t % 2], ("xb", t % 2))
    S.fence()
    A.release()


def store_x(c, s):
    S = c.S
    S.fence()
    for t in range(NT):
        S.dma("sp", c.dr["out"][s * SEQ + t * 128: s * SEQ + (t + 1) * 128, :], c.x[:, t, :], reads=[("x", t)])
    S.fence()


def make_xT_tile(c, t, xb, xbres):
    S = c.S
    x, xT, PT, ident = c.x, c.xT, c.PT, c.ident
    S.copy("act", xb[:, :], x[:, t, :], [("x", t)], [xbres])
    for k in range(8):
        S.tr(PT[:, k * 128:(k + 1) * 128], xb[:, k * 128:(k + 1) * 128], ident[:], [xbres, "ident"], ["PT"])
    S.copy("dve", xT[:, :, t * 128:(t + 1) * 128], PT[:, :].rearrange("p (a b) -> p a b", b=128), ["PT"], [("xT", t)])


def layer_norm_all(c, gname, bname, l, out_rows):
    S, A, x = c.S, c.A, c.x
    A.mark()
    st = A.get([128, NT, 12], F32)
    mv = A.get([128, NT, 2], F32)
    vv = A.get([128, NT], F32)
    rstd = A.get([128, NT], F32)
    gb = A.get([128, D], F32)
    bb = A.get([128, D], F32)
    xb = [A.get([128, D], BF16) for _ in range(2)]
    S.dma("sp", gb, c.dr[gname][l].partition_broadcast(128), writes=["ln_g"])
    S.dma("sp", bb, c.dr[bname][l].partition_broadcast(128), writes=["ln_b"])

    def bnst(o, i, r, w):
        S.op("dve", lambda e: e.bn_stats(out=o, in_=i), r, w)

    def bnag(o, i, r, w):
        S.op("dve", lambda e: e.bn_aggr(out=o, in_=i), r, w)
    for t in range(NT):
        bnst(st[:, t, 0:6], x[:, t, 0:512], [("x", t)], [("st", t, 0)])
        bnst(st[:, t, 6:12], x[:, t, 512:1024], [("x", t)], [("st", t, 1)])
        bnag(mv[:, t, :], st[:, t, :], [("st", t, 0), ("st", t, 1)], [("mv", t)])
    S.ts("dve", vv[:, :], mv[:, :, 1], LN_EPS, None, ALU.add, None, [("mv", t) for t in range(NT)], ["vv"])
    S.tt("pool", rstd[:, :], vv[:, :], c.mhalf[:, :], ALU.pow, ["vv", "mhalf"], ["rstd"])
    for t in range(NT):
        S.ts("dve", x[:, t, :], x[:, t, :], mv[:, t, 0:1], rstd[:, t:t + 1], ALU.subtract, ALU.mult,
             [("x", t), ("mv", t), "rstd"], [("x", t)])
        S.tt("pool", x[:, t, :], x[:, t, :], gb, ALU.mult, [("x", t), "ln_g"], [("x", t)])
        S.tt("pool", x[:, t, :], x[:, t, :], bb, ALU.add, [("x", t), "ln_b"], [("x", t)])
        if out_rows is not None:
            S.dma("sp", c.dr["out"][out_rows + t * 128: out_rows + (t + 1) * 128, :], x[:, t, :], reads=[("x", t)])
        else:
            make_xT_tile(c, t, xb[t % 2], ("xb", t % 2))
    S.fence()
    A.release()


HBLOCKS = [(0, 4), (4, 4), (8, 4), (12, 4), (16, 4), (20, 2)]


def ffn(c, l, wname, lnname, out_rows):
    S, A, x, xT, P = c.S, c.A, c.x, c.xT, c.P
    S.fence()
    A.mark()
    w1d = c.dr[wname + "_w1"][l].rearrange("(c p) h -> p c h", p=128)
    w3d = c.dr[wname + "_w3"][l].rearrange("(c p) h -> p c h", p=128)
    w2d = c.dr[wname + "_w2"][l]
    W1 = [A.get([128, 8, 512], BF16) for _ in range(2)]
    W3 = [A.get([128, 8, 512], BF16) for _ in range(2)]
    W2 = [A.get([128, 4, D], BF16) for _ in range(2)]
    G = [A.get([128, 4, 512], BF16) for _ in range(2)]
    TMP = [A.get([128, 512], F32) for _ in range(2)]

    def load(i):
        c0, n = HBLOCKS[i]
        s = i % 2
        for kq in range(4):
            S.dma("pool", W1[s][:, 2 * kq:2 * kq + 2, 0:n * 128], w1d[:, 2 * kq:2 * kq + 2, c0 * 128:(c0 + n) * 128], writes=[("w1", s, kq)])
        for kq in range(4):
            S.dma("pool", W3[s][:, 2 * kq:2 * kq + 2, 0:n * 128], w3d[:, 2 * kq:2 * kq + 2, c0 * 128:(c0 + n) * 128], writes=[("w3", s, kq)])
        for j in range(n):
            S.dma("pool", W2[s][:, j, :], w2d[(c0 + j) * 128:(c0 + j + 1) * 128, :], writes=[("w2", s, j)])

    load(0)
    load(1)
    for t in range(NT):
        S.ts("pool", x[:, t, :], x[:, t, :], ALPHA, None, ALU.mult, None, [("x", t)], [("x", t)])
    gi = 0
    pi = 0
    for i, (c0, n) in enumerate(HBLOCKS):
        s = i % 2
        for tb in range(4):
            g = G[gi % 2]
            gres = ("g", gi % 2)
            gi += 1
            tsl = slice(tb * 512, (tb + 1) * 512)
            xres = [("xT", tb * 4 + q) for q in range(4)]
            for j in range(n):
                b1, b3 = (pi % 2) * 2, (pi % 2) * 2 + 1
                tmp, tres = TMP[pi % 2], ("tmp", pi % 2)
                pi += 1
                for k in range(8):
                    S.mm(P[b1][:, :], W1[s][:, k, j * 128:(j + 1) * 128], xT[:, k, tsl], k == 0, k == 7,
                         [("w1", s, k // 2)] + xres, [("P", b1)])
                for k in range(8):
                    S.mm(P[b3][:, :], W3[s][:, k, j * 128:(j + 1) * 128], xT[:, k, tsl], k == 0, k == 7,
                         [("w3", s, k // 2)] + xres, [("P", b3)])
                S.act(tmp, P[b1][:, :], AF.Silu, [("P", b1)], [tres])
                S.tt("dve", g[:, j, :], tmp, P[b3][:, :], ALU.mult, [tres, ("P", b3)], [gres])
            for tt_ in range(4):
                t = tb * 4 + tt_
                for half in range(2):
                    hs = slice(half * 512, (half + 1) * 512)
                    for j in range(n):
                        S.mm(P[4 + half][:, :], g[:, j, tt_ * 128:(tt_ + 1) * 128], W2[s][:, j, hs], j == 0, j == n - 1,
                             [gres, ("w2", s, j)], [("P", 4 + half)])
                    S.stt(x[:, t, hs], P[4 + half][:, :], 0.5, x[:, t, hs], ALU.mult, ALU.add,
                          [("P", 4 + half), ("x", t)], [("x", t)])
        if i + 2 < len(HBLOCKS):
            load(i + 2)
    S.fence()
    A.release()
    layer_norm_all(c, lnname + "_g", lnname + "_b", l, out_rows)


class Pair:
    def __init__(self, smm, nq, scale, pv, rows=128, post=None):
        self.smm, self.nq, self.scale, self.pv, self.rows, self.post = smm, nq, scale, pv, rows, post


def run_pairs(c, pairs, func=AF.Exp):
    S, P = c.S, c.P
    n = len(pairs)

    def emit_score(i):
        p = pairs[i]
        b = i % 2
        m = len(p.smm)
        for idx, (lhsT, rhs, a, bb, rd) in enumerate(p.smm):
            S.mm(P[b][0:p.rows, a:bb], lhsT, rhs, idx == 0, idx == m - 1, rd, [("P", b)])
    if n:
        emit_score(0)
    for i in range(n):
        if i + 1 < n:
            emit_score(i + 1)
        p = pairs[i]
        e = c.eT[i % 3]
        eres = ("eT", i % 3)
        S.act(e[0:p.rows, 0:p.nq], P[i % 2][0:p.rows, 0:p.nq], func, [("P", i % 2)], [eres], scale=p.scale)
        for (a, bb, vrhs, outap, st, sp, rd, wr) in p.pv:
            S.mm(outap, e[0:p.rows, a:bb], vrhs, st, sp, [eres] + rd, wr, skip=True)
        if p.post is not None:
            p.post()


def nextbank(c):
    b = 4 + c.gb % 3
    c.gb += 1
    return b


def rope_combine(c, b1, b2, RC, RS, dst, dres, rows=128, r0=0):
    S, P = c.S, c.P
    rs = slice(r0, r0 + rows)
    S.tt("dve", c.t1[rs, :], P[b1][rs, :], RC[rs, :], ALU.mult, [("P", b1), "RC"], ["t1"])
    S.tt("dve", c.t2[rs, :], P[b2][rs, :], RS[rs, :], ALU.mult, [("P", b2), "RS"], ["t2"])
    S.tt("pool", dst, c.t1[rs, :], c.t2[rs, :], ALU.add, ["t1", "t2"], [dres])


def mixer(c, l, phases):
    S, A, x = c.S, c.A, c.x
    groups = [g for g in ("mla", "nsa", "dsa", "sb") if g in phases]
    if not groups:
        return
    S.fence()
    A.mark()
    c.gb = 0
    c.gn = A.get([128, D], F32)
    S.dma("sp", c.gn, c.dr["group_norm_g"][l].partition_broadcast(128), writes=["gn"])
    c.wo = A.get([128, 2, D], BF16)
    c.ob = [A.get([128, 4, 256], F32)] * 2
    c.ynb = A.get([128, 4, 256], BF16)
    c.ynT = A.get([128, 8, 128], BF16)
    c.junk = A.get([128, 256], BF16)
    c.gst = A.get([128, 16], F32)
    c.eT = [A.get([128, 512], BF16) for _ in range(3)]
    c.t1 = A.get([128, 512], F32)
    c.t2 = A.get([128, 512], F32)
    c.rden = [A.get([128, 8], F32) for _ in range(2)]
    for t in range(NT):
        S.ts("pool", x[:, t, :], x[:, t, :], ALPHA, 0.0, ALU.mult, ALU.add, [("x", t)], [("x", t)])
    fns = {"mla": mixer_mla, "nsa": mixer_nsa, "dsa": mixer_dsa, "sb": mixer_sb}
    for gi, g in enumerate(("mla", "nsa", "dsa", "sb")):
        if g in groups:
            S.dma("pool", c.wo, c.dr["w_out"][l][gi * 256:(gi + 1) * 256, :].rearrange("(c p) d -> p c d", p=128), writes=["wo"])
            fns[g](c, l)
            S.fence()
    A.release()
    layer_norm_all(c, "ln2_g", "ln2_b", l, None)


def mix_finish(c, l, gidx, qb, o, ores):
    S, P, PT, x = c.S, c.P, c.PT, c.x
    gst = c.gst
    tapname = f"y_{'abcd'[gidx]}_{l}"
    if tapname in c.taps:
        for j in range(4):
            t = 4 * qb + j
            S.dma("sp", c.taps[tapname][t * 128:(t + 1) * 128, :], o[:, j, :], reads=[ores])
    for j in range(4):
        S.stt(c.junk[:, :], o[:, j, :], 1.0, o[:, j, :], ALU.mult, ALU.mult, [ores], ["junk", "gst_a"], accum=gst[:, j:j + 1])
    S.ts("dve", gst[:, 4:8], gst[:, 0:4], 1.0 / 256.0, RMS_EPS, ALU.mult, ALU.add, ["gst_a"], ["gst_b"])
    S.tt("pool", gst[:, 8:12], gst[:, 4:8], c.mhalf[:, 0:4], ALU.pow, ["gst_b", "mhalf"], ["gst_c"])
    for j in range(4):
        S.stt(c.ynb[:, j, :], o[:, j, :], gst[:, 8 + j:9 + j], c.gn[:, gidx * 256:(gidx + 1) * 256], ALU.mult, ALU.mult,
              [ores, "gst_c", "gn"], ["ynb"])
    for j in range(4):
        for cc in range(2):
            S.tr(PT[:, (2 * j + cc) * 128:(2 * j + cc + 1) * 128], c.ynb[:, j, cc * 128:(cc + 1) * 128], c.ident[:],
                 ["ynb", "ident"], ["PT"])
    S.copy("act", c.ynT[:, :, :], PT[:, :].rearrange("p (a b) -> p a b", b=128), ["PT"], ["ynT"])
    for j in range(4):
        t = 4 * qb + j
        for half in range(2):
            hs = slice(half * 512, (half + 1) * 512)
            b = nextbank(c)
            for cc in range(2):
                S.mm(P[b][:, :], c.ynT[:, 2 * j + cc, :], c.wo[:, cc, hs], cc == 0, cc == 1, ["ynT", "wo"], [("P", b)])
            S.tt("dve", x[:, t, hs], x[:, t, hs], P[b][:, :], ALU.add, [("x", t), ("P", b)], [("x", t)])


def finish_group_o(c, ob, rd, o, ores, h, width=64, stride=128, den_col=64):
    S, P = c.S, c.P
    for j in range(4):
        S.recip(rd[:, j:j + 1], P[ob][:, j * stride + den_col:j * stride + den_col + 1], [("P", ob)], [("rden", id(rd), j)])
        S.ts("dve", o[:, j, h * 64:(h + 1) * 64], P[ob][:, j * stride:j * stride + width], rd[:, j:j + 1], None, ALU.mult, None,
             [("P", ob), ("rden", id(rd), j)], [ores])


def mixer_mla(c, l):
    S, A, P, PT, xT, dr = c.S, c.A, c.P, c.PT, c.xT, c.dr
    A.mark()
    scale = float(96 ** -0.5)
    winv = dr["w_in"][l].rearrange("(c p) n -> p c n", p=128)
    win = A.get([128, 8, 384], BF16)
    wkr = A.get([128, 8, 128], BF16)
    wkr_f = A.get([128, 8, 32], F32)
    wq_f = A.get([128, 2, 384], F32)
    wq = A.get([128, 2, 512], BF16)
    wkv_f = A.get([128, 512], F32)
    wkv = A.get([128, 512], BF16)
    qg = A.get([128, 2], F32)
    kvg = A.get([128, 1], F32)
    S.dma("pool", win, winv[:, :, 0:384], writes=["win"])
    S.dma("sp", wkr_f, winv[:, :, 384:416], writes=["wkr_f"])
    S.dma("sp", wq_f, dr["mla_w_uq"][l].rearrange("(c p) n -> p c n", p=128), writes=["wq_f"])
    S.dma("sp", wkv_f, dr["mla_w_ukv"][l], writes=["wkv_f"])
    for cc in range(2):
        S.dma("sp", qg[:, cc:cc + 1], dr["mla_q_norm"][l][cc * 128:(cc + 1) * 128].rearrange("(p o) -> p o", o=1), writes=["qg"])
    S.dma("sp", kvg[:, 0:1], dr["mla_kv_norm"][l].rearrange("(p o) -> p o", o=1), writes=["kvg"])
    for r in range(2):
        S.copy("pool", wkr[:, :, r * 32:(r + 1) * 32], wkr_f[:, :, :], ["wkr_f"], ["wkr"])
        S.ts("pool", wkr[:, :, 64 + r * 32:64 + r * 32 + 16], wkr_f[:, :, 16:32], -1.0, 0.0, ALU.mult, ALU.add, ["wkr_f"], ["wkr"])
        S.copy("pool", wkr[:, :, 64 + r * 32 + 16:64 + r * 32 + 32], wkr_f[:, :, 0:16], ["wkr_f"], ["wkr"])
    for cc in range(2):
        src = wq_f[:, cc, :].rearrange("p (h e) -> p h e", e=96)
        g1 = qg[:, cc:cc + 1]
        S.ts("pool", wq[:, cc, 0:256].rearrange("p (h e) -> p h e", e=64), src[:, :, 0:64], g1, 1.0, ALU.mult, ALU.mult, ["wq_f", "qg"], ["wq"])
        S.ts("pool", wq[:, cc, 256:384].rearrange("p (h e) -> p h e", e=32), src[:, :, 64:96], g1, 1.0, ALU.mult, ALU.mult, ["wq_f", "qg"], ["wq"])
        rotv = wq[:, cc, 384:512].rearrange("p (h e) -> p h e", e=32)
        S.ts("pool", rotv[:, :, 0:16], src[:, :, 80:96], g1, -1.0, ALU.mult, ALU.mult, ["wq_f", "qg"], ["wq"])
        S.ts("pool", rotv[:, :, 16:32], src[:, :, 64:80], g1, 1.0, ALU.mult, ALU.mult, ["wq_f", "qg"], ["wq"])
    srckv = wkv_f[:, :].rearrange("p (h e) -> p h e", e=128)
    S.ts("pool", wkv[:, 0:256].rearrange("p (h e) -> p h e", e=64), srckv[:, :, 0:64], kvg[:, 0:1], 1.0, ALU.mult, ALU.mult, ["wkv_f", "kvg"], ["wkv"])
    S.ts("pool", wkv[:, 256:512].rearrange("p (h e) -> p h e", e=64), srckv[:, :, 64:128], kvg[:, 0:1], 1.0, ALU.mult, ALU.mult, ["wkv_f", "kvg"], ["wkv"])
    cT = A.get([128, 3, SEQ], BF16)
    cn = [A.get([128, 384], BF16) for _ in range(2)]
    ssq = A.get([128, NT, 2], F32)
    vv2 = A.get([128, NT, 2], F32)
    rs2 = A.get([128, NT, 2], F32)
    for tt in range(NT):
        b = nextbank(c)
        tsl = slice(tt * 128, (tt + 1) * 128)
        for k in range(8):
            S.mm(P[b][:, 0:384], xT[:, k, tsl], win[:, k, 0:384], k == 0, k == 7, [("xT", tt), "win"], [("P", b)])
        S.act(c.junk[:, 0:256], P[b][:, 0:256], AF.Square, [("P", b)], ["junk"], accum=ssq[:, tt, 0:1])
        S.act(c.junk[:, 0:128], P[b][:, 256:384], AF.Square, [("P", b)], ["junk"], accum=ssq[:, tt, 1:2])
        S.ts("dve", vv2[:, tt, 0:1], ssq[:, tt, 0:1], 1.0 / 256.0, RMS_EPS, ALU.mult, ALU.add, ["junk"], [("vv2", tt)])
        S.ts("dve", vv2[:, tt, 1:2], ssq[:, tt, 1:2], 1.0 / 128.0, RMS_EPS, ALU.mult, ALU.add, ["junk"], [("vv2", tt)])
        S.tt("pool", rs2[:, tt, :], vv2[:, tt, :], c.mhalf[:, 0:2], ALU.pow, [("vv2", tt), "mhalf"], [("rs2", tt)])
        cnb = cn[tt % 2]
        cres = ("cn", tt % 2)
        S.ts("dve", cnb[:, 0:256], P[b][:, 0:256], rs2[:, tt, 0:1], None, ALU.mult, None, [("P", b), ("rs2", tt)], [cres])
        S.ts("dve", cnb[:, 256:384], P[b][:, 256:384], rs2[:, tt, 1:2], None, ALU.mult, None, [("P", b), ("rs2", tt)], [cres])
        for cc in range(3):
            S.tr(PT[:, cc * 128:(cc + 1) * 128], cnb[:, cc * 128:(cc + 1) * 128], c.ident[:], [cres, "ident"], ["PT"])
        S.copy("act", cT[:, :, tsl], PT[:, 0:384].rearrange("p (a b) -> p a b", b=128), ["PT"], [("cT", tt)])
    QN = [A.get([128, SEQ], BF16) for _ in range(2)]
    KN = [A.get([128, SEQ], BF16) for _ in range(2)]
    QPE = [A.get([128, SEQ], BF16) for _ in range(2)]
    KPE = A.get([128, SEQ], BF16)
    RC = A.get([128, 512], F32)
    RS = A.get([128, 512], F32)
    for tb in range(4):
        tsl = slice(tb * 512, (tb + 1) * 512)
        cres = [("cT", tb * 4 + q) for q in range(4)]
        xres = [("xT", tb * 4 + q) for q in range(4)]
        S.dma("sp", RC, dr["c_rope32"][0][:, tsl], writes=["RC"])
        S.dma("sp", RS, dr["c_rope32"][1][:, tsl], writes=["RS"])
        for hp in range(2):
            b = nextbank(c)
            for cc in range(2):
                S.mm(P[b][:, :], wq[:, cc, hp * 128:(hp + 1) * 128], cT[:, cc, tsl], cc == 0, cc == 1, ["wq"] + cres, [("P", b)])
            S.copy("act", QN[hp][:, tsl], P[b][:, :], [("P", b)], [("QN", hp, tb)])
        for hp in range(2):
            b1 = nextbank(c)
            for cc in range(2):
                S.mm(P[b1][0:64, :], wq[:, cc, 256 + hp * 64:320 + hp * 64], cT[:, cc, tsl], cc == 0, cc == 1, ["wq"] + cres, [("P", b1)])
            b2 = nextbank(c)
            for cc in range(2):
                S.mm(P[b2][0:64, :], wq[:, cc, 384 + hp * 64:448 + hp * 64], cT[:, cc, tsl], cc == 0, cc == 1, ["wq"] + cres, [("P", b2)])
            rope_combine(c, b1, b2, RC, RS, QPE[hp][0:64, tsl], ("QPE", hp, tb), rows=64)
        for hp in range(2):
            b = nextbank(c)
            S.mm(P[b][:, :], wkv[:, hp * 128:(hp + 1) * 128], cT[:, 2, tsl], True, True, ["wkv"] + cres, [("P", b)])
            S.copy("act", KN[hp][:, tsl], P[b][:, :], [("P", b)], [("KN", hp, tb)])
        b1 = nextbank(c)
        for k in range(8):
            S.mm(P[b1][0:64, :], wkr[:, k, 0:64], xT[:, k, tsl], k == 0, k == 7, ["wkr"] + xres, [("P", b1)])
        b2 = nextbank(c)
        for k in range(8):
            S.mm(P[b2][0:64, :], wkr[:, k, 64:128], xT[:, k, tsl], k == 0, k == 7, ["wkr"] + xres, [("P", b2)])
        rope_combine(c, b1, b2, RC, RS, KPE[0:64, tsl], ("KPE", tb), rows=64)
    V = A.get([128, NT, 260], BF16)
    S.memset("pool", V[:, :, :], 1.0, ["V"])
    for tt in range(NT):
        b = nextbank(c)
        S.mm(P[b][:, 0:256], cT[:, 2, tt * 128:(tt + 1) * 128], wkv[:, 256:512], True, True, [("cT", tt), "wkv"], [("P", b)])
        S.copy("dve", V[:, tt, :].rearrange("p (h e) -> p h e", e=65)[:, :, 0:64], P[b][:, 0:256].rearrange("p (h e) -> p h e", e=64),
               [("P", b), "V"], [("V", tt)])
    negtri = c.masks[:, 0, :]
    gcount = 0
    for qb in range(4):
        o = c.ob[0]
        ores = "ob"
        pairs = []
        for h in range(4):
            hp, r0 = h // 2, 64 * (h % 2)
            ob = 2 + gcount % 2
            rd = c.rden[gcount % 2]
            gcount += 1
            nk = 4 * qb + 4
            for kt in range(nk):
                d = kt - 4 * qb
                n0 = max(0, d) * 128
                q0 = qb * 512 + n0
                nq = 512 - n0
                ksl = slice(kt * 128, (kt + 1) * 128)
                qres = [("QN", hp, qb), ("QPE", hp, qb), ("KN", hp, kt // 4), ("KPE", kt // 4)]
                p0 = 32 * (h % 2)
                smm = [(KN[hp][r0:r0 + 64, ksl], QN[hp][r0:r0 + 64, q0:q0 + nq], 0, nq, qres),
                       (KPE[p0:p0 + 32, ksl], QPE[hp][p0:p0 + 32, q0:q0 + nq], 0, nq, qres)]
                if d >= 0:
                    smm.append((c.ident[:], negtri, 0, 128, ["ident", "masks"]))
                pv = []
                for j in range(max(0, d), 4):
                    pv.append((j * 128 - n0, (j + 1) * 128 - n0, V[:, kt, h * 65:(h + 1) * 65], P[ob][:, j * 128:j * 128 + 65],
                               kt == 0 and j == 0, kt == 4 * qb + j, [("V", kt)], [("P", ob)]))
                post = None
                if kt == nk - 1:
                    def post(ob=ob, rd=rd, o=o, ores=ores, h=h, qb=qb):
                        finish_group_o(c, ob, rd, o, ores, h)
                        if h == 3:
                            mix_finish(c, l, 0, qb, o, ores)
                pairs.append(Pair(smm, nq, scale, pv, post=post))
        run_pairs(c, pairs)
    A.release()


def mixer_nsa(c, l):
    S, A, P, PT, xT, dr = c.S, c.A, c.P, c.PT, c.xT, c.dr
    A.mark()
    winv = dr["w_in"][l].rearrange("(c p) n -> p c n", p=128)
    o0 = OFF["nsa_q"]
    QT = [A.get([128, SEQ], BF16) for _ in range(2)]
    KS2 = A.get([128, SEQ], BF16)
    KW2 = A.get([128, SEQ], BF16)
    VS = A.get([128, NT, 65], BF16)
    VW = A.get([128, NT, 65], BF16)
    gates = A.get([128, NT, 12], F32)
    kcT2 = A.get([128, 128], BF16)
    vca = A.get([128, 97], BF16)
    NST = [A.get([32, 512], BF16) for _ in range(2)]
    imp = A.get([128, 4, 32], F32)
    impm = A.get([128, 4, 32], F32)
    nsel = A.get([128, 4, 32], BF16)
    top8 = A.get([128, 4, 8], F32)
    gd = A.get([128, 16], F32)
    A.mark()
    win = A.get([128, 8, 652], BF16)
    S.dma("pool", win, winv[:, :, o0:o0 + 652], writes=["win"])
    wq_r = A.get([128, 8, 256], BF16)
    rot_cols(S, "pool", wq_r[:, :, :].rearrange("p c (h e) -> p c h e", e=64), win[:, :, 0:256].rearrange("p c (h e) -> p c h e", e=64), 32, ["win"], ["wq_r"])
    wks = A.get([128, 8, 256], BF16)
    wkw = A.get([128, 8, 256], BF16)
    wkc = A.get([128, 8, 128], BF16)
    for (wt, off, nm) in ((wks, 384, "wks"), (wkw, 512, "wkw")):
        for r in range(2):
            S.copy("pool", wt[:, :, r * 64:(r + 1) * 64], win[:, :, off:off + 64], ["win"], [nm])
            S.ts("pool", wt[:, :, 128 + r * 64:128 + r * 64 + 32], win[:, :, off + 32:off + 64], -1.0, 0.0, ALU.mult, ALU.add, ["win"], [nm])
            S.copy("pool", wt[:, :, 128 + r * 64 + 32:128 + (r + 1) * 64], win[:, :, off:off + 32], ["win"], [nm])
    S.copy("pool", wkc[:, :, 0:64], win[:, :, 256:320], ["win"], ["wkc"])
    S.ts("pool", wkc[:, :, 64:96], win[:, :, 288:320], -1.0, 0.0, ALU.mult, ALU.add, ["win"], ["wkc"])
    S.copy("pool", wkc[:, :, 96:128], win[:, :, 256:288], ["win"], ["wkc"])
    w1k = A.get([64, 32, 64], BF16)
    w1v = A.get([64, 32, 64], BF16)
    w2k = A.get([64, 128], BF16)
    w2v = A.get([64, 64], BF16)
    posf = A.get([32, 64], BF16)
    posT = A.get([64, 32], BF16)
    S.dma("pool", w1k, dr["nsa_cmp_wk1"][l].rearrange("(i e) o -> e i o", e=64), writes=["w1k"])
    S.dma("pool", w1v, dr["nsa_cmp_wv1"][l].rearrange("(i e) o -> e i o", e=64), writes=["w1v"])
    S.dma("pool", w2k[:, 0:64], dr["nsa_cmp_wk2"][l], writes=["w2k"])
    S.dma("pool", w2k[:, 64:128], dr["nsa_cmp_wk2"][l], writes=["w2k"])
    S.dma("pool", w2v, dr["nsa_cmp_wv2"][l], writes=["w2v"])
    S.dma("pool", posf, dr["nsa_cmp_pos"][l], writes=["posf"])
    S.tr(PT[0:64, 0:32], posf[:, :], c.ident[0:32, 0:32], ["posf", "ident"], ["PT"])
    S.copy("dve", posT[:, :], PT[0:64, 0:32], ["PT"], ["posT"])
    KCT = A.get([64, SEQ], BF16)
    VCT = A.get([64, SEQ], BF16)
    RC = A.get([128, 512], F32)
    RS = A.get([128, 512], F32)
    for tb in range(4):
        tsl = slice(tb * 512, (tb + 1) * 512)
        xres = [("xT", tb * 4 + q) for q in range(4)]
        S.dma("sp", RC, dr["c_rope64"][0][:, tsl], writes=["RC"])
        S.dma("sp", RS, dr["c_rope64"][1][:, tsl], writes=["RS"])
        for hp in range(2):
            b1, b2 = proj_fm(c, [(win[:, :, hp * 128:(hp + 1) * 128], "win", 128), (wq_r[:, :, hp * 128:(hp + 1) * 128], "wq_r", 128)], tsl, xres)
            rope_combine(c, b1, b2, RC, RS, QT[hp][:, tsl], ("QT", hp, tb))
        b1, b2 = proj_fm(c, [(wks[:, :, 0:128], "wks", 128), (wks[:, :, 128:256], "wks", 128)], tsl, xres)
        rope_combine(c, b1, b2, RC, RS, KS2[:, tsl], ("KS2", tb))
        b1, b2 = proj_fm(c, [(wkw[:, :, 0:128], "wkw", 128), (wkw[:, :, 128:256], "wkw", 128)], tsl, xres)
        rope_combine(c, b1, b2, RC, RS, KW2[:, tsl], ("KW2", tb))
        b1, b2 = proj_fm(c, [(wkc[:, :, 0:64], "wkc", 64), (wkc[:, :, 64:128], "wkc", 64)], tsl, xres)
        rope_combine(c, b1, b2, RC, RS, KCT[:, tsl], ("KCT", tb), rows=64)
        (b1,) = proj_fm(c, [(win[:, :, 320:384], "win", 64)], tsl, xres)
        S.copy("act", VCT[:, tsl], P[b1][0:64, :], [("P", b1)], [("VCT", tb)])
    S.memset("pool", VS[:, :, :], 1.0, ["VS"])
    S.memset("pool", VW[:, :, :], 1.0, ["VW"])
    for tt in range(NT):
        b = nextbank(c)
        for k in range(8):
            S.mm(P[b][:, 0:204], xT[:, k, tt * 128:(tt + 1) * 128], win[:, k, 448:652], k == 0, k == 7, [("xT", tt), "win"], [("P", b)])
        S.copy("dve", VS[:, tt, 0:64], P[b][:, 0:64], [("P", b), "VS"], [("VS", tt)])
        S.copy("dve", VW[:, tt, 0:64], P[b][:, 128:192], [("P", b), "VW"], [("VW", tt)])
        S.act(gates[:, tt, :], P[b][:, 192:204], AF.Sigmoid, [("P", b)], [("gates", tt)])
    kall = [("KCT", tb) for tb in range(4)]
    vall = [("VCT", tb) for tb in range(4)]
    cb = A.get([64, 2], F32)
    gact = A.get([64, 2, 128], BF16)
    gtmp = A.get([64, 4, 128], F32)
    for vi, (w1, w1n, ZT, zres) in enumerate(((w1k, "w1k", KCT, kall), (w1v, "w1v", VCT, vall))):
        b = nextbank(c)
        for i in range(32):
            S.mm(P[b][0:64, 0:1], w1[:, i, :], posT[:, i:i + 1], i == 0, i == 31, [w1n, "posT"], [("P", b)])
        S.copy("dve", cb[:, vi:vi + 1], P[b][0:64, 0:1], [("P", b)], [("cb", vi)])
        b = nextbank(c)
        for i in range(32):
            S.mm(P[b][0:64, 0:127], w1[:, i, :], ZT[:, i:i + 16 * 126 + 1:16], i == 0, i == 31, [w1n] + zres, [("P", b)])
        u = gtmp[:, 0, 0:127]
        S.ts("dve", u, P[b][0:64, 0:127], cb[:, vi:vi + 1], None, ALU.add, None, [("P", b), ("cb", vi)], ["g_u"])
        S.tt("dve", gtmp[:, 1, 0:127], u, u, ALU.mult, ["g_u"], ["g_u2"])
        S.ts("dve", gtmp[:, 1, 0:127], gtmp[:, 1, 0:127], 0.044715, 1.0, ALU.mult, ALU.add, ["g_u2"], ["g_u2"])
        S.tt("dve", gtmp[:, 2, 0:127], gtmp[:, 1, 0:127], u, ALU.mult, ["g_u2", "g_u"], ["g_in"])
        S.act(gtmp[:, 3, 0:127], gtmp[:, 2, 0:127], AF.Sigmoid, ["g_in"], ["g_sg"], scale=float(2.0 * (2.0 / np.pi) ** 0.5))
        S.tt("dve", gact[:, vi, 0:127], gtmp[:, 3, 0:127], u, ALU.mult, ["g_sg", "g_u"], [("gact", vi)])
    S.memset("pool", kcT2[:, :], 0.0, ["kcT2"])
    b = nextbank(c)
    S.mm(P[b][:, 0:127], w2k[:, :], gact[:, 0, 0:127], True, True, ["w2k", ("gact", 0)], [("P", b)])
    S.copy("dve", kcT2[:, 0:127], P[b][:, 0:127], [("P", b), "kcT2"], ["kcT2"])
    S.memset("pool", vca[:, :], 1.0, ["vca"])
    S.dma("pool", vca[:, 64:96], dr["c_overlap"], reads=["vca"], writes=["vca"])
    b = nextbank(c)
    S.mm(P[b][0:127, 0:64], gact[:, 1, 0:127], w2v[:, :], True, True, ["w2v", ("gact", 1)], [("P", b)])
    S.copy("dve", vca[0:127, 0:64], P[b][0:127, 0:64], [("P", b), "vca"], ["vca"])
    S.fence()
    A.release()
    negtri = c.masks[:, 0, :]
    negwin = c.masks[:, 2, :]
    o = c.ob[0]
    ores = "ob"
    gcount = 0
    gall = [("gates", t) for t in range(NT)]
    for qb in range(4):
        qbs = slice(qb * 512, (qb + 1) * 512)
        nst = NST[qb % 2]
        nres = ("nst", qb % 2)
        pairs = []
        for h in range(4):
            hp, r0 = h // 2, 64 * (h % 2)
            ob = 2 + gcount % 2
            rd = c.rden[gcount % 2]
            gcount += 1
            smm = [(kcT2[r0:r0 + 64, 0:127], QT[hp][r0:r0 + 64, qbs], 0, 512, ["kcT2", ("QT", hp, qb)]),
                   (c.ident[:, 0:127], c.cmpmask[:, qbs], 0, 512, ["ident", "cmpmask"])]
            pv = [(j * 128, (j + 1) * 128, vca[0:127, 0:97], P[ob][:, j * 128:j * 128 + 97], j == 0, True, ["vca"], [("P", ob)]) for j in range(4)]

            def post(ob=ob, rd=rd, h=h, qb=qb):
                for j in range(4):
                    t = 4 * qb + j
                    S.ts("dve", rd[:, j:j + 1], P[ob][:, j * 128 + 96:j * 128 + 97], 1e-30, None, ALU.max, None, [("P", ob)], [("rden", id(rd), j)])
                    S.recip(rd[:, j:j + 1], rd[:, j:j + 1], [("rden", id(rd), j)], [("rden", id(rd), j)])
                    S.tt("dve", rd[:, 4 + j:5 + j], rd[:, j:j + 1], gates[:, t, h:h + 1], ALU.mult, [("rden", id(rd), j)] + gall, [("rdg", id(rd), j)])
                    S.ts("dve", o[:, j, h * 64:(h + 1) * 64], P[ob][:, j * 128:j * 128 + 64], rd[:, 4 + j:5 + j], None, ALU.mult, None,
                         [("P", ob), ("rdg", id(rd), j)], [ores])
                    if h == 0:
                        S.ts("dve", imp[:, j, :], P[ob][:, j * 128 + 64:j * 128 + 96], rd[:, j:j + 1], None, ALU.mult, None,
                             [("P", ob), ("rden", id(rd), j)], ["imp"])
                    else:
                        S.stt(imp[:, j, :], P[ob][:, j * 128 + 64:j * 128 + 96], rd[:, j:j + 1], imp[:, j, :], ALU.mult, ALU.add,
                              [("P", ob), ("rden", id(rd), j), "imp"], ["imp"])
            pairs.append(Pair(smm, 512, 0.125, pv, rows=127, post=post))
        run_pairs(c, pairs)
        S.tt("dve", impm[:, :, :], imp[:, :, :], c.selkeep[:, 4 * qb:4 * qb + 4, :], ALU.mult, ["imp", "selkeep"], ["impm"])
        S.tt("dve", impm[:, :, :], impm[:, :, :], c.seladd[:, 4 * qb:4 * qb + 4, :], ALU.add, ["impm", "seladd"], ["impm"])
        for j in range(4):
            S.op("dve", (lambda o_, i_: (lambda e: e.max(out=o_, in_=i_)))(top8[:, j, :], impm[:, j, :]), ["impm"], [("top8", j)])
            S.ts("dve", nsel[:, j, :], impm[:, j, :], top8[:, j, 7:8], -BIG, ALU.is_lt, ALU.mult, ["impm", ("top8", j)], [("nsel", j)])
            S.tr(PT[0:32, j * 128:(j + 1) * 128], nsel[:, j, :], c.ident[:], [("nsel", j), "ident"], ["PT"])
        S.copy("dve", nst[:, :], PT[0:32, 0:512], ["PT"], [nres])
        for branch in (1, 2):
            pairs = []
            for h in range(4):
                hp, r0 = h // 2, 64 * (h % 2)
                ob = 2 + gcount % 2
                rd = c.rden[gcount % 2]
                gcount += 1
                kts = list(range(4 * qb + 4)) if branch == 1 else list(range(max(0, 4 * qb - 4), 4 * qb + 4))
                first = True
                for kt in kts:
                    ksl = slice(kt * 128, (kt + 1) * 128)
                    d = kt - 4 * qb
                    if d >= 0:
                        n0, n1 = d * 128, 512
                        jr = range(d, 4)
                    elif branch == 2:
                        i = kt - (4 * qb - 4)
                        n0, n1 = 0, 128 * (i + 1)
                        jr = range(0, i + 1)
                    else:
                        n0, n1 = 0, 512
                        jr = range(0, 4)
                    nq = n1 - n0
                    q0 = qb * 512 + n0
                    KX, kname, VX, vname = (KS2, "KS2", VS, "VS") if branch == 1 else (KW2, "KW2", VW, "VW")
                    smm = [(KX[r0:r0 + 64, ksl], QT[hp][r0:r0 + 64, q0:q0 + nq], 0, nq, [(kname, kt // 4), ("QT", hp, qb)])]
                    if branch == 1:
                        smm.append((c.expand[:, kt, :], nst[:, n0:n1], 0, nq, ["expand", nres]))
                    if d >= 0:
                        smm.append((c.ident[:], negtri, 0, 128, ["ident", "masks"]))
                    elif branch == 2:
                        smm.append((c.ident[:], negwin, n1 - 128 - n0, n1 - n0, ["ident", "masks"]))
                    pv = []
                    for j in jr:
                        pv.append((j * 128 - n0, (j + 1) * 128 - n0, VX[:, kt, :], P[ob][:, j * 128:j * 128 + 65], first, kt == 4 * qb + j,
                                   [(vname, kt)], [("P", ob)]))
                        first = False
                    post = None
                    if kt == kts[-1]:
                        def post(ob=ob, rd=rd, h=h, qb=qb, branch=branch):
                            for j in range(4):
                                t = 4 * qb + j
                                S.recip(rd[:, j:j + 1], P[ob][:, j * 128 + 64:j * 128 + 65], [("P", ob)], [("rden", id(rd), j)])
                                S.tt("dve", rd[:, 4 + j:5 + j], rd[:, j:j + 1], gates[:, t, branch * 4 + h:branch * 4 + h + 1], ALU.mult,
                                     [("rden", id(rd), j)] + gall, [("rdg", id(rd), j)])
                                S.stt(o[:, j, h * 64:(h + 1) * 64], P[ob][:, j * 128:j * 128 + 64], rd[:, 4 + j:5 + j], o[:, j, h * 64:(h + 1) * 64],
                                      ALU.mult, ALU.add, [("P", ob), ("rdg", id(rd), j), ores], [ores])
                            if branch == 2 and h == 3:
                                mix_finish(c, l, 1, qb, o, ores)
                    pairs.append(Pair(smm, nq, 0.125, pv, post=post))
            run_pairs(c, pairs)
    A.release()


def rot_cols(S, eng, dst, src, half, reads, writes):
    S.ts(eng, dst[:, :, :, 0:half], src[:, :, :, half:2 * half], -1.0, 0.0, ALU.mult, ALU.add, reads, writes)
    S.copy(eng, dst[:, :, :, half:2 * half], src[:, :, :, 0:half], reads, writes)


def proj_fm(c, wlist, tsl, xres):
    S, P, xT = c.S, c.P, c.xT
    banks = []
    for (w, wres, m) in wlist:
        b = nextbank(c)
        for k in range(8):
            S.mm(P[b][0:m, :], w[:, k, :], xT[:, k, tsl], k == 0, k == 7, [wres] + xres, [("P", b)])
        banks.append(b)
    return banks


def mixer_dsa(c, l):
    S, A, P, PT, xT, dr = c.S, c.A, c.P, c.PT, c.xT, c.dr
    A.mark()
    winv = dr["w_in"][l].rearrange("(c p) n -> p c n", p=128)
    o0 = OFF["dsa_q"]
    QT = [A.get([128, SEQ], BF16) for _ in range(2)]
    KT2 = A.get([128, SEQ], BF16)
    IQ = [A.get([128, SEQ], BF16) for _ in range(3)]
    IK = A.get([128, SEQ], BF16)
    V = A.get([128, NT, 65], BF16)
    wraw = A.get([128, NT, 8], F32)
    absw = A.get([128, NT, 8], F32)
    sgn = A.get([128, NT, 8], F32)
    A.mark()
    win = A.get([128, 8, 680], BF16)
    S.dma("pool", win, winv[:, :, o0:o0 + 680], writes=["win"])
    wq_r = A.get([128, 8, 256], BF16)
    rot_cols(S, "pool", wq_r[:, :, :].rearrange("p c (h e) -> p c h e", e=64), win[:, :, 0:256].rearrange("p c (h e) -> p c h e", e=64), 32, ["win"], ["wq_r"])
    wk2 = A.get([128, 8, 256], BF16)
    for r in range(2):
        S.copy("pool", wk2[:, :, r * 64:(r + 1) * 64], win[:, :, 256:320], ["win"], ["wk2"])
        S.ts("pool", wk2[:, :, 128 + r * 64:128 + r * 64 + 32], win[:, :, 288:320], -1.0, 0.0, ALU.mult, ALU.add, ["win"], ["wk2"])
        S.copy("pool", wk2[:, :, 128 + r * 64 + 32:128 + (r + 1) * 64], win[:, :, 256:288], ["win"], ["wk2"])
    wiq_r = A.get([128, 8, 256], BF16)
    rot_cols(S, "pool", wiq_r[:, :, :].rearrange("p c (h e) -> p c h e", e=32), win[:, :, 384:640].rearrange("p c (h e) -> p c h e", e=32), 16, ["win"], ["wiq_r"])
    wik3 = A.get([128, 8, 192], BF16)
    for r in range(3):
        S.copy("pool", wik3[:, :, r * 32:(r + 1) * 32], win[:, :, 640:672], ["win"], ["wik3"])
        S.ts("pool", wik3[:, :, 96 + r * 32:96 + r * 32 + 16], win[:, :, 656:672], -1.0, 0.0, ALU.mult, ALU.add, ["win"], ["wik3"])
        S.copy("pool", wik3[:, :, 96 + r * 32 + 16:96 + (r + 1) * 32], win[:, :, 640:656], ["win"], ["wik3"])
    RC = A.get([128, 512], F32)
    RS = A.get([128, 512], F32)
    RC2 = A.get([128, 512], F32)
    RS2 = A.get([128, 512], F32)
    for tb in range(4):
        tsl = slice(tb * 512, (tb + 1) * 512)
        xres = [("xT", tb * 4 + q) for q in range(4)]
        S.dma("sp", RC, dr["c_rope64"][0][:, tsl], writes=["RC"])
        S.dma("sp", RS, dr["c_rope64"][1][:, tsl], writes=["RS"])
        S.dma("sp", RC2, dr["c_rope32"][0][:, tsl], writes=["RC2"])
        S.dma("sp", RS2, dr["c_rope32"][1][:, tsl], writes=["RS2"])
        for hp in range(2):
            b1, b2 = proj_fm(c, [(win[:, :, hp * 128:(hp + 1) * 128], "win", 128), (wq_r[:, :, hp * 128:(hp + 1) * 128], "wq_r", 128)], tsl, xres)
            rope_combine(c, b1, b2, RC, RS, QT[hp][:, tsl], ("QT", hp, tb))
        b1, b2 = proj_fm(c, [(wk2[:, :, 0:128], "wk2", 128), (wk2[:, :, 128:256], "wk2", 128)], tsl, xres)
        rope_combine(c, b1, b2, RC, RS, KT2[:, tsl], ("KT2", tb))
        for ti in range(3):
            nh = 3 if ti < 2 else 2
            m = 32 * nh
            b1, b2 = proj_fm(c, [(win[:, :, 384 + ti * 96:384 + ti * 96 + m], "win", m), (wiq_r[:, :, ti * 96:ti * 96 + m], "wiq_r", m)], tsl, xres)
            rope_combine2(c, b1, b2, RC2, RS2, IQ[ti][0:m, tsl], ("IQ", ti, tb), m)
        b1, b2 = proj_fm(c, [(wik3[:, :, 0:96], "wik3", 96), (wik3[:, :, 96:192], "wik3", 96)], tsl, xres)
        rope_combine2(c, b1, b2, RC2, RS2, IK[0:96, tsl], ("IK", tb), 96)
    S.memset("pool", V[:, :, :], 1.0, ["V"])
    for tt in range(NT):
        b = nextbank(c)
        for k in range(8):
            S.mm(P[b][:, 0:64], xT[:, k, tt * 128:(tt + 1) * 128], win[:, k, 320:384], k == 0, k == 7, [("xT", tt), "win"], [("P", b)])
        S.copy("dve", V[:, tt, 0:64], P[b][:, 0:64], [("P", b), "V"], [("V", tt)])
        b = nextbank(c)
        for k in range(8):
            S.mm(P[b][:, 0:8], xT[:, k, tt * 128:(tt + 1) * 128], win[:, k, 672:680], k == 0, k == 7, [("xT", tt), "win"], [("P", b)])
        S.copy("dve", wraw[:, tt, :], P[b][:, 0:8], [("P", b)], [("wraw", tt)])
    wr_all = [("wraw", tt) for tt in range(NT)]
    S.ts("dve", sgn[:, :, :], wraw[:, :, :], 0.0, 2.0, ALU.is_ge, ALU.mult, wr_all, ["sgn"])
    S.ts("dve", sgn[:, :, :], sgn[:, :, :], -1.0, None, ALU.add, None, ["sgn"], ["sgn"])
    S.tt("dve", absw[:, :, :], wraw[:, :, :], sgn[:, :, :], ALU.mult, wr_all + ["sgn"], ["absw"])
    S.ts("dve", absw[:, :, :], absw[:, :, :], 0.0625, None, ALU.mult, None, ["absw"], ["absw"])
    S.fence()
    A.release()
    SC = [A.get([128, SEQ], F32) for _ in range(2)]
    NEGM = [A.get([128, SEQ], BF16) for _ in range(2)]
    RT = [A.get([128, 512], F32) for _ in range(2)]
    JK = A.get([128, SEQ], BF16)
    sm = A.get([128, 64], F32)
    WT = A.get([128, NIT], F32)
    o = c.ob[0]
    ores = "ob"
    rcnt = 0
    gcount = 0
    for qt in range(NT):
        L = 128 * (qt + 1)
        sc = SC[qt % 2]
        scres = ("sc", qt % 2)
        negm = NEGM[qt % 2]
        nmres = ("negm", qt % 2)
        qsl = slice(qt * 128, (qt + 1) * 128)
        nkb = (L + 511) // 512
        for kb in range(nkb):
            w = min(512, L - kb * 512)
            for h in range(8):
                ti, base = h // 3, 32 * (h % 3)
                b = nextbank(c)
                S.mm(P[b][:, 0:w], IQ[ti][base:base + 32, qsl], IK[base:base + 32, kb * 512:kb * 512 + w], True, True,
                     [("IQ", ti, qt // 4), ("IK", kb)], [("P", b)])
                rt = RT[rcnt % 2]
                rres = ("rt", rcnt % 2)
                rcnt += 1
                S.act(rt[:, 0:w], P[b][:, 0:w], AF.Relu, [("P", b), "absw"], [rres], scale=absw[:, qt, h:h + 1])
                if h == 0:
                    S.ts("dve", sc[:, kb * 512:kb * 512 + w], rt[:, 0:w], sgn[:, qt, 0:1], None, ALU.mult, None, [rres, "sgn"], [scres])
                else:
                    S.stt(sc[:, kb * 512:kb * 512 + w], rt[:, 0:w], sgn[:, qt, h:h + 1], sc[:, kb * 512:kb * 512 + w], ALU.mult, ALU.add,
                          [rres, "sgn", scres], [scres])
        lo = sm[:, 3:4]
        if qt >= 2:
            S.op("dve", (lambda o_, i_: (lambda e: e.tensor_reduce(out=o_, in_=i_, axis=mybir.AxisListType.X, op=ALU.max)))(sm[:, 0:1], sc[:, 0:L]), [scres], ["rmax"])
            S.op("dve", (lambda o_, i_: (lambda e: e.tensor_reduce(out=o_, in_=i_, axis=mybir.AxisListType.X, op=ALU.min)))(sm[:, 1:2], sc[:, 0:L]), [scres], ["rmin"])
        S.tt("dve", sc[:, qsl], sc[:, qsl], c.negtrif[:, :], ALU.add, [scres, "negtrif"], [scres])
        if qt >= 2:
            S.tt("dve", sm[:, 2:3], sm[:, 0:1], sm[:, 1:2], ALU.subtract, ["rmax", "rmin"], ["w0"])
            S.ts("dve", WT[:, :], c.pow2[:, :], sm[:, 2:3], None, ALU.mult, None, ["pow2", "w0"], ["WT"])
            S.copy("dve", lo, sm[:, 1:2], ["rmin"], ["lo"])
            for it in range(NIT):
                S.tt("dve", sm[:, 5:6], lo, WT[:, it:it + 1], ALU.add, ["lo", "WT"], ["mid"])
                S.ts("dve", JK[:, 0:L], sc[:, 0:L], sm[:, 5:6], None, ALU.is_ge, ALU.add, [scres, "mid"], ["JK", "cnt"], accum=sm[:, 6:7])
                S.ts("dve", sm[:, 7:8], sm[:, 6:7], 256.0, WT[:, it:it + 1], ALU.is_ge, ALU.mult, ["cnt", "WT"], ["step"])
                S.tt("dve", lo, lo, sm[:, 7:8], ALU.add, ["lo", "step"], ["lo"])
        else:
            S.memset("dve", lo, -1e29, ["lo"])
        S.ts("dve", negm[:, 0:L], sc[:, 0:L], lo, -BIG, ALU.is_lt, ALU.mult, [scres, "lo"], [nmres])
        ob = 2 + gcount % 2
        rd = c.rden[gcount % 2]
        gcount += 1
        pairs = []
        for kt in range(qt + 1):
            ksl = slice(kt * 128, (kt + 1) * 128)
            smm = []
            for h in range(4):
                hp, r0 = h // 2, 64 * (h % 2)
                smm.append((KT2[r0:r0 + 64, ksl], QT[hp][r0:r0 + 64, qsl], h * 128, (h + 1) * 128, [("KT2", kt // 4), ("QT", hp, qt // 4)]))
                smm.append((negm[:, ksl], c.ident[:], h * 128, (h + 1) * 128, [nmres, "ident"]))
            pv = []
            for h in range(4):
                pv.append((h * 128, (h + 1) * 128, V[:, kt, :], P[ob][:, h * 128:h * 128 + 65], kt == 0 and h == 0, kt == qt, [("V", kt)], [("P", ob)]))
            post = None
            if kt == qt:
                def post(ob=ob, rd=rd, qt=qt):
                    j = qt % 4
                    for h in range(4):
                        S.recip(rd[:, h:h + 1], P[ob][:, h * 128 + 64:h * 128 + 65], [("P", ob)], [("rden", id(rd), h)])
                        S.ts("dve", o[:, j, h * 64:(h + 1) * 64], P[ob][:, h * 128:h * 128 + 64], rd[:, h:h + 1], None, ALU.mult, None,
                             [("P", ob), ("rden", id(rd), h)], [ores])
                    if j == 3:
                        mix_finish(c, l, 2, qt // 4, o, ores)
            pairs.append(Pair(smm, 512, 0.125, pv, post=post))
        run_pairs(c, pairs)
    A.release()


def rope_combine2(c, b1, b2, RC, RS, dst, dres, rows):
    S, P = c.S, c.P
    S.tt("dve", c.t1[0:rows, :], P[b1][0:rows, :], RC[0:rows, :], ALU.mult, [("P", b1), "RC2"], ["t1"])
    S.tt("dve", c.t2[0:rows, :], P[b2][0:rows, :], RS[0:rows, :], ALU.mult, [("P", b2), "RS2"], ["t2"])
    S.tt("pool", dst, c.t1[0:rows, :], c.t2[0:rows, :], ALU.add, ["t1", "t2"], [dres])


def mixer_sb(c, l):
    S, A, P, xT, dr = c.S, c.A, c.P, c.xT, c.dr
    A.mark()
    winv = dr["w_in"][l].rearrange("(c p) n -> p c n", p=128)
    o0 = OFF["sb_q"]
    win = A.get([128, 8, 768], BF16)
    S.dma("pool", win, winv[:, :, o0:o0 + 768], writes=["win"])
    QT = [A.get([128, SEQ], BF16) for _ in range(2)]
    KT = [A.get([128, SEQ], BF16) for _ in range(2)]
    V = A.get([128, NT, 256], BF16)
    SPM = A.get([128, 16, 512], BF16)
    E1 = [A.get([128, 512], F32) for _ in range(2)]
    for tb in range(4):
        tsl = slice(tb * 512, (tb + 1) * 512)
        xres = [("xT", tb * 4 + q) for q in range(4)]
        for hp in range(2):
            b = nextbank(c)
            for k in range(8):
                S.mm(P[b][:, :], win[:, k, hp * 128:(hp + 1) * 128], xT[:, k, tsl], k == 0, k == 7, ["win"] + xres, [("P", b)])
            S.act(QT[hp][:, tsl], P[b][:, :], AF.Copy, [("P", b)], [("QT", hp, tb)], scale=0.125)
            b = nextbank(c)
            for k in range(8):
                S.mm(P[b][:, :], win[:, k, 256 + hp * 128:256 + (hp + 1) * 128], xT[:, k, tsl], k == 0, k == 7, ["win"] + xres, [("P", b)])
            S.copy("dve", KT[hp][:, tsl], P[b][:, :], [("P", b)], [("KT", hp, tb)])
    for tt in range(NT):
        b = nextbank(c)
        for k in range(8):
            S.mm(P[b][:, 0:256], xT[:, k, tt * 128:(tt + 1) * 128], win[:, k, 512:768], k == 0, k == 7, [("xT", tt), "win"], [("P", b)])
        S.copy("dve", V[:, tt, :], P[b][:, 0:256], [("P", b)], [("V", tt)])
    negstrict = c.masks[:, 1, :]
    uge = c.cums[:, 0, :]
    nones = c.cums[:, 1, :]
    cnt = 0
    gcount = 0
    o = c.ob[0]
    ores = "ob"
    for qb in range(4):
        nk = 4 * qb + 4
        for h in range(4):
            hp, r0 = h // 2, 64 * (h % 2)
            for kt in range(nk):
                d = kt - 4 * qb
                n0 = max(0, d) * 128
                q0 = qb * 512 + n0
                nq = 512 - n0
                ksl = slice(kt * 128, (kt + 1) * 128)
                b = cnt % 2
                e1 = E1[cnt % 2]
                cnt += 1
                qres = [("QT", hp, qb), ("KT", hp, kt // 4)]
                S.mm(P[b][:, 0:nq], KT[hp][r0:r0 + 64, ksl], QT[hp][r0:r0 + 64, q0:q0 + nq], True, d < 0, qres, [("P", b)])
                if d >= 0:
                    S.mm(P[b][:, 0:128], c.ident[:], negstrict, False, True, ["ident", "masks"], [("P", b)])
                S.act(e1[:, 0:nq], P[b][:, 0:nq], AF.Exp, [("P", b)], [("e1", b)])
                S.act(SPM[:, kt, n0:512], e1[:, 0:nq], AF.Ln, [("e1", b)], [("spm", kt)], bias=c.one[:, 0:1])
            ob = 2 + gcount % 2
            gcount += 1
            pairs = []
            for ks in range(nk):
                d = ks - 4 * qb
                n0 = max(0, d) * 128
                q0 = qb * 512 + n0
                nq = 512 - n0
                ksl = slice(ks * 128, (ks + 1) * 128)
                qres = [("QT", hp, qb), ("KT", hp, ks // 4)]
                smm = [(KT[hp][r0:r0 + 64, ksl], QT[hp][r0:r0 + 64, q0:q0 + nq], 0, nq, qres)]
                for kj in range(ks + 1, nk):
                    n0j = max(0, kj - 4 * qb) * 128
                    smm.append((nones, SPM[:, kj, n0j:512], n0j - n0, nq, ["cums", ("spm", kj)]))
                smm.append((uge, SPM[:, ks, n0:512], 0, nq, ["cums", ("spm", ks)]))
                if d >= 0:
                    smm.append((c.ident[:], negstrict, 0, 128, ["ident", "masks"]))
                pv = []
                for j in range(max(0, d), 4):
                    pv.append((j * 128 - n0, (j + 1) * 128 - n0, V[:, ks, h * 64:(h + 1) * 64], P[ob][:, j * 64:(j + 1) * 64],
                               ks == 0 and j == 0, ks == 4 * qb + j, [("V", ks)], [("P", ob)]))
                post = None
                if ks == nk - 1:
                    def post(ob=ob, h=h, qb=qb):
                        S.copy("dve", o[:, :, h * 64:(h + 1) * 64], P[ob][:, 0:256].rearrange("p (a b) -> p a b", b=64), [("P", ob)], [ores])
                        if h == 3:
                            mix_finish(c, l, 3, qb, o, ores)
                pairs.append(Pair(smm, nq, 1.0, pv, post=post))
            run_pairs(c, pairs)
    A.release()


_CACHE = {}


def kernel(**inputs):
    x = np.ascontiguousarray(np.asarray(inputs["x"], dtype=np.float32))
    B = x.shape[0]
    per = 2
    if "nc" not in _CACHE:
        _CACHE["nc"] = build(nseq=per)
    nc = _CACHE["nc"]
    consts = make_consts()
    shared = {k: np.ascontiguousarray(np.asarray(inputs[k], dtype=np.float32)) for k in WEIGHT_SHAPES}
    shared.update(consts)
    outs = []
    ncl = CORES_PER_LAUNCH
    for g in range(0, B // per, ncl):
        in_maps = []
        for i in range(g, g + ncl):
            m = dict(shared)
            m["x"] = x[i * per:(i + 1) * per].reshape(per * SEQ, D)
            in_maps.append(m)
        res = run_bass_kernel_spmd(nc, in_maps, core_ids=list(range(ncl)))
        outs.extend(r["out"].reshape(per, SEQ, D) for r in res.results)
    out = np.concatenate(outs, axis=0)
    return out.astype(np.float32)
```
